# Optimizing a Trainium2 kernel written in Bass

```python
import jax, jax.numpy as jnp
from jax import lax
import numpy as np

D_MODEL = 1024
BATCH = 2
SEQ = 8192
DEPTH = 2

N_MIXERS = 2
GLA_HEADS = 4
GLA_KEY_DIM = D_MODEL // 2
GLA_VAL_DIM = D_MODEL
GLA_HEAD_K = GLA_KEY_DIM // GLA_HEADS
GLA_HEAD_V = GLA_VAL_DIM // GLA_HEADS
GLA_GATE_RANK = 16
GLA_GATE_TAU = 16.0
GLA_CHUNK = 64
GLA_IN_DIM = 2 * GLA_KEY_DIM + 2 * GLA_VAL_DIM + GLA_GATE_RANK
SC_WIDTH = D_MODEL
CONV_WIDTH = 3
D_FF = ((8 * D_MODEL // 3 + 255) // 256) * 256
N_GLA = (DEPTH + 1) // 2
N_SC = DEPTH // 2
RMS_EPS = 1e-6

kernel_name = "hybrid_gla_shortconv_convffn"


def rmsnorm(x, g):
    x32 = x.astype(jnp.float32)
    y = x32 * lax.rsqrt(jnp.mean(x32 * x32, axis=-1, keepdims=True) + RMS_EPS)
    return y.astype(x.dtype) * g


def causal_dwconv(x, w):
    T = x.shape[1]
    K = w.shape[1]
    xp = jnp.pad(x, ((0, 0), (K - 1, 0), (0, 0)))
    y = xp[:, 0:T] * w[:, 0]
    for j in range(1, K):
        y = y + xp[:, j:j + T] * w[:, j]
    return y


def to_chunks(t, n_heads, head_dim):
    B, T, _ = t.shape
    n = T // GLA_CHUNK
    return t.reshape(B, n, GLA_CHUNK, n_heads, head_dim).transpose(0, 3, 1, 2, 4)


def gla_mixer(h, w_in, w_gate_up, b_gate, head_norm_g, w_out):
    B, T, _ = h.shape
    proj = h @ w_in
    s1 = GLA_KEY_DIM
    s2 = 2 * GLA_KEY_DIM
    s3 = s2 + GLA_VAL_DIM
    s4 = s3 + GLA_VAL_DIM
    q, k, v, g, a_low = proj[..., :s1], proj[..., s1:s2], proj[..., s2:s3], proj[..., s3:s4], proj[..., s4:]
    log_a = jax.nn.log_sigmoid((a_low @ w_gate_up + b_gate).astype(jnp.float32)) / GLA_GATE_TAU

    qc = to_chunks(q.astype(jnp.float32) * (GLA_HEAD_K ** -0.5), GLA_HEADS, GLA_HEAD_K)
    kc = to_chunks(k.astype(jnp.float32), GLA_HEADS, GLA_HEAD_K)
    vc = to_chunks(v.astype(jnp.float32), GLA_HEADS, GLA_HEAD_V)
    lc = to_chunks(log_a, GLA_HEADS, GLA_HEAD_K)

    bcum = jnp.cumsum(lc, axis=3)
    b_last = bcum[..., -1, :]
    q_in = qc * jnp.exp(bcum)
    k_in = kc * jnp.exp(-bcum)
    mask = jnp.tril(jnp.ones((GLA_CHUNK, GLA_CHUNK), dtype=bool))
    scores = jnp.where(mask, jnp.einsum('bhncd,bhnsd->bhncs', q_in, k_in), 0.0)
    o_intra = jnp.einsum('bhncs,bhnse->bhnce', scores, vc)
    k_dec = kc * jnp.exp(b_last[..., None, :] - bcum)
    chunk_kv = jnp.einsum('bhncd,bhnce->bhnde', k_dec, vc)

    def step(S, inp):
        q_n, kv_n, dec_n = inp
        o_n = jnp.einsum('bhcd,bhde->bhce', q_n, S)
        S = dec_n[..., None] * S + kv_n
        return S, o_n

    S0 = jnp.zeros((B, GLA_HEADS, GLA_HEAD_K, GLA_HEAD_V), jnp.float32)
    xs = (q_in.transpose(2, 0, 1, 3, 4), chunk_kv.transpose(2, 0, 1, 3, 4), jnp.exp(b_last).transpose(2, 0, 1, 3))
    _, o_inter = lax.scan(step, S0, xs)
    o = o_intra + o_inter.transpose(1, 2, 0, 3, 4)
    o = o.transpose(0, 2, 3, 1, 4).reshape(B, T, GLA_HEADS, GLA_HEAD_V)
    o = o * lax.rsqrt(jnp.mean(o * o, axis=-1, keepdims=True) + RMS_EPS)
    o = o.astype(h.dtype) * head_norm_g
    o = o.reshape(B, T, GLA_VAL_DIM) * jax.nn.silu(g)
    return o @ w_out


def short_conv_mixer(h, w_in, conv_w, w_out):
    proj = h @ w_in
    b, c, u = proj[..., :SC_WIDTH], proj[..., SC_WIDTH:2 * SC_WIDTH], proj[..., 2 * SC_WIDTH:]
    y = b * causal_dwconv(c * u, conv_w)
    return y @ w_out


def conv_ffn(h, w_up, conv_w, w_down):
    up = causal_dwconv(h @ w_up, conv_w)
    a, u = up[..., :D_FF], up[..., D_FF:]
    return (jax.nn.silu(a) * u) @ w_down


def setup_inputs(seed: int = 0) -> dict:
    key = jax.random.key(seed)
    ks = jax.random.split(key, 16)
    f32 = jnp.float32
    out_scale = (2.0 * DEPTH) ** -0.5
    nrm = lambda k, shape, s: jax.random.normal(k, shape, f32) * s
    return {
        "x": nrm(ks[0], (BATCH, SEQ, D_MODEL), 1.0),
        "norm_mix_g": 1.0 + nrm(ks[1], (DEPTH, D_MODEL), 0.02),
        "norm_ffn_g": 1.0 + nrm(ks[2], (DEPTH, D_MODEL), 0.02),
        "gla_w_in": nrm(ks[3], (N_GLA, D_MODEL, GLA_IN_DIM), D_MODEL ** -0.5),
        "gla_w_gate_up": nrm(ks[4], (N_GLA, GLA_GATE_RANK, GLA_KEY_DIM), GLA_GATE_RANK ** -0.5),
        "gla_b_gate": nrm(ks[5], (N_GLA, GLA_KEY_DIM), 0.1),
        "gla_head_norm_g": 1.0 + nrm(ks[6], (N_GLA, GLA_HEADS, GLA_HEAD_V), 0.02),
        "gla_w_out": nrm(ks[7], (N_GLA, GLA_VAL_DIM, D_MODEL), GLA_VAL_DIM ** -0.5 * out_scale),
        "sc_w_in": nrm(ks[8], (N_SC, D_MODEL, 3 * SC_WIDTH), D_MODEL ** -0.5),
        "sc_conv_w": nrm(ks[9], (N_SC, SC_WIDTH, CONV_WIDTH), CONV_WIDTH ** -0.5),
        "sc_w_out": nrm(ks[10], (N_SC, SC_WIDTH, D_MODEL), SC_WIDTH ** -0.5 * out_scale),
        "ffn_w_up": nrm(ks[11], (DEPTH, D_MODEL, 2 * D_FF), D_MODEL ** -0.5),
        "ffn_conv_w": nrm(ks[12], (DEPTH, 2 * D_FF, CONV_WIDTH), CONV_WIDTH ** -0.5),
        "ffn_w_down": nrm(ks[13], (DEPTH, D_FF, D_MODEL), D_FF ** -0.5 * out_scale),
        "final_norm_g": 1.0 + nrm(ks[14], (D_MODEL,), 0.02),
    }


def reference(x, norm_mix_g, norm_ffn_g, gla_w_in, gla_w_gate_up, gla_b_gate, gla_head_norm_g, gla_w_out,
              sc_w_in, sc_conv_w, sc_w_out, ffn_w_up, ffn_conv_w, ffn_w_down, final_norm_g):
    for i in range(DEPTH):
        h = rmsnorm(x, norm_mix_g[i])
        j = i // N_MIXERS
        if i % N_MIXERS == 0:
            x = x + gla_mixer(h, gla_w_in[j], gla_w_gate_up[j], gla_b_gate[j], gla_head_norm_g[j], gla_w_out[j])
        else:
            x = x + short_conv_mixer(h, sc_w_in[j], sc_conv_w[j], sc_w_out[j])
        h = rmsnorm(x, norm_ffn_g[i])
        x = x + conv_ffn(h, ffn_w_up[i], ffn_conv_w[i], ffn_w_down[i])
    return rmsnorm(x, final_norm_g)
```

```python
import numpy as np
import concourse.bass as bass
import concourse.mybir as mybir
from concourse.bass_utils import run_bass_kernel_spmd

F32 = mybir.dt.float32
BF16 = mybir.dt.bfloat16
AF = mybir.ActivationFunctionType
ALU = mybir.AluOpType

NCORES = 8
TOK = 2048
NT = 16
D = 1024
DFF = 2816
EPS = 1e-6
ENGINES = ("pe", "act", "dve", "pool", "sp")


class Op:
    __slots__ = ("eng", "fn", "deps", "signaled", "sem", "val", "is_dma", "inc")

    def __init__(self, eng, fn, is_dma=False, inc=1):
        self.eng = eng
        self.fn = fn
        self.deps = []
        self.signaled = False
        self.sem = None
        self.val = None
        self.is_dma = is_dma
        self.inc = inc


class Prog:
    def __init__(self, nc, n_dma_sems=24):
        self.nc = nc
        self.streams = {e: [] for e in ENGINES}
        self.tiles = {}
        self.esem = {e: nc.alloc_semaphore(name=f"s_{e}") for e in ENGINES}
        self.dsems = [nc.alloc_semaphore(name=f"s_dma{i}") for i in range(n_dma_sems)]
        self.dcnt = [0] * n_dma_sems
        self.dlast = [None] * n_dma_sems
        self.drr = 0
        self.pending = {e: [] for e in ENGINES}

    def _track(self, op, reads, writes):
        deps = []
        for r in reads:
            t = self.tiles.setdefault(r, {"w": None, "r": []})
            if t["w"] is not None:
                deps.append((t["w"], True))
        for w in writes:
            t = self.tiles.setdefault(w, {"w": None, "r": []})
            if t["w"] is not None:
                deps.append((t["w"], False))
            for r in t["r"]:
                deps.append((r, False))
        for d in self.pending[op.eng]:
            deps.append((d, True))
        self.pending[op.eng] = []
        for d, raw in deps:
            if d is op:
                continue
            if d.eng == op.eng and not d.is_dma and not op.is_dma:
                if op.eng == "pe" or not raw:
                    continue
            if d not in op.deps:
                op.deps.append(d)
                d.signaled = True
        for r in reads:
            self.tiles[r]["r"].append(op)
        for w in writes:
            t = self.tiles[w]
            t["w"] = op
            t["r"] = []

    def op(self, eng, fn, reads=(), writes=()):
        o = Op(eng, fn)
        self._track(o, reads, writes)
        self.streams[eng].append(o)
        return o

    def dma(self, eng, fn, reads=(), writes=(), inc=16):
        o = Op(eng, fn, is_dma=True, inc=inc)
        i = self.drr
        self.drr = (self.drr + 1) % len(self.dsems)
        o.sem = self.dsems[i]
        self.dcnt[i] += inc
        o.val = self.dcnt[i]
        o.signaled = True
        if self.dlast[i] is not None:
            o.deps.append(self.dlast[i])
        self.dlast[i] = o
        self._track(o, reads, writes)
        self.streams[eng].append(o)
        return o

    def barrier(self):
        lasts = []
        for e in ENGINES:
            for o in reversed(self.streams[e]):
                if not o.is_dma:
                    lasts.append(o)
                    break
        lasts += [o for o in self.dlast if o is not None]
        for e in ENGINES:
            self.pending[e] = [o for o in lasts if not (o.eng == e and not o.is_dma)]

    def emit(self, final_waits=()):
        nc = self.nc
        for e in ENGINES:
            c = 0
            for o in self.streams[e]:
                if o.is_dma:
                    continue
                if o.signaled:
                    c += 1
                    o.sem = self.esem[e]
                    o.val = c
        engobj = {"pe": "tensor", "act": "scalar", "dve": "vector", "pool": "gpsimd", "sp": "sync"}
        with nc.Block() as block:
            for e in ENGINES:
                ops = self.streams[e]
                if not ops and not (e == "sp" and final_waits):
                    continue

                def body(eng, ops=ops, e=e):
                    waited = {}
                    for o in ops:
                        for d in o.deps:
                            k = id(d.sem)
                            if waited.get(k, 0) >= d.val:
                                continue
                            waited[k] = d.val
                            eng.wait_ge(d.sem, d.val)
                        ins = o.fn(eng)
                        if o.signaled:
                            ins.then_inc(o.sem, o.inc)
                    if e == "sp":
                        for d in final_waits:
                            if waited.get(id(d.sem), 0) >= d.val:
                                continue
                            waited[id(d.sem)] = d.val
                            eng.wait_ge(d.sem, d.val)

                getattr(block, engobj[e])(body)


class Builder:
    SB_LO = 16512
    SB_HI = 229344

    def __init__(self, stages=4):
        self.stages = stages
        nc = bass.Bass("TRN2", target_bir_lowering=False)
        self.nc = nc
        self.P = Prog(nc)
        dt = lambda name, shape: nc.dram_tensor(name, list(shape), F32, kind="ExternalInput").ap()
        self.x_d = dt("x", [TOK, D])
        self.norm_mix_g = dt("norm_mix_g", [2, D])
        self.norm_ffn_g = dt("norm_ffn_g", [2, D])
        self.gla_w_in = dt("gla_w_in", [1, D, 3088])
        self.gla_w_gate_up = dt("gla_w_gate_up", [1, 16, 512])
        self.gla_b_gate = dt("gla_b_gate", [1, 512])
        self.gla_hn_g = dt("gla_head_norm_g", [1, 4, 256])
        self.gla_w_out = dt("gla_w_out", [1, D, D])
        self.sc_w_in = dt("sc_w_in", [1, D, 3072])
        self.sc_conv_w = dt("sc_conv_w", [1, D, 3])
        self.sc_w_out = dt("sc_w_out", [1, D, D])
        self.ffn_w_up = dt("ffn_w_up", [2, D, 2 * DFF])
        self.ffn_conv_w = dt("ffn_conv_w", [2, 2 * DFF, 3])
        self.ffn_w_down = dt("ffn_w_down", [2, DFF, D])
        self.final_g = dt("final_norm_g", [1, D])
        self.cst_d = dt("cst", [128, 640])
        self.out_d = nc.dram_tensor("out", [TOK, D], F32, kind="ExternalOutput").ap()
        self.cc_st_in = nc.dram_tensor("cc_st_in", [128, 1028], F32).ap()
        self.cc_st_out = nc.dram_tensor("cc_st_out", [NCORES * 128, 1028], F32).ap()
        self.cc_h_in = [nc.dram_tensor(f"cc_h_in{i}", [2, D], F32).ap() for i in range(3)]
        self.cc_h_out = [nc.dram_tensor(f"cc_h_out{i}", [NCORES * 2, D], F32).ap() for i in range(3)]

        self.sb_ptr = self.SB_LO
        self.uid = 0
        self.X = self.sb("X", [128, NT, D], F32)
        self.cst = self.sb("cst", [128, 640], F32)
        self.identb = self.sb("identb", [128, 128], BF16)
        self.selb = self.sb("selb", [16, 2], BF16)
        self.gbc = self.sb("gbc", [128, D], F32)
        self.ss = self.sb("ss", [128, NT], F32)
        self.rstd = self.sb("rstd", [128, NT], F32)
        self.convw = self.sb("convw", [128, 44, 3], F32)
        self.small = self.sb("small", [128, 64], F32)
        self.junk = self.sb("junk", [128, D], BF16)
        self.arena_lo = self.sb_ptr
        self.psf = [nc.alloc_psum_tensor(f"psf{i}", [128, 512], F32).ap() for i in range(5)]
        self.halo_bank = (nc.alloc_psum_tensor("pshalo", [128, 512], F32).ap(), "pshalo")
        self.psb = [nc.alloc_psum_tensor(f"psb{i}", [128, 1024], BF16).ap() for i in range(2)]
        self.psf_i = 0
        self.psb_i = 0
        self.rot = None
        self.rot_default = [(self.psf[i], f"psf{i}") for i in range(5)]

    def sb(self, name, shape, dtype, at=None):
        nbytes = int(np.prod(shape[1:])) * (4 if dtype == F32 else 2)
        if at is None:
            off = (self.sb_ptr + 31) // 32 * 32
            self.sb_ptr = off + nbytes
        else:
            off = at
        assert off + nbytes <= self.SB_HI, f"SBUF overflow {name}: {off + nbytes - self.SB_HI}"
        self.uid += 1
        return self.nc.alloc_sbuf_tensor_at(f"{name}_{self.uid}", list(shape), dtype, offset=off).ap()

    def arena_reset(self):
        self.sb_ptr = self.arena_lo

    def fbank(self):
        rot = self.rot if self.rot is not None else self.rot_default
        i = self.psf_i % len(rot)
        self.psf_i = (i + 1) % len(rot)
        return rot[i]

    def fbank2(self):
        a = self.fbank()
        b = self.fbank()
        return a, b

    def bbank(self):
        i = self.psb_i
        self.psb_i = (i + 1) % len(self.psb)
        return self.psb[i], f"psb{i}"

    def load_consts(self):
        P = self.P
        P.dma("sp", lambda e: e.dma_start(out=self.cst, in_=self.cst_d), writes=["cst"])
        P.op("dve", lambda e: e.tensor_copy(out=self.identb, in_=self.cst[:, 0:128]), reads=["cst"], writes=["identb"])
        P.op("dve", lambda e: e.tensor_copy(out=self.selb, in_=self.cst[0:16, 528:530]), reads=["cst"], writes=["selb"])
        self.tri_incl = self.cst[:, 128:256]
        self.tri_rev = self.cst[:, 256:384]
        self.cmask = self.cst[:, 384:512]
        self.negcol = self.cst[:, 512:513]
        self.mj = self.cst[:, 513:521]
        self.omj = self.cst[:, 536:544]

    def load_x(self):
        xv = self.x_d.rearrange("(n p) d -> p n d", p=128)
        for i in range(NT):
            self.P.dma("sp", lambda e, i=i: e.dma_start(out=self.X[:, i, :], in_=xv[:, i, :]), writes=[f"x{i}"])

    def load_gbc(self, g_row_ap):
        self.P.dma("sp", lambda e: e.dma_start(out=self.gbc, in_=g_row_ap.partition_broadcast(128)), writes=["gbc"])

    def norm_stats(self):
        P = self.P
        for i in range(NT):
            P.op("act", lambda e, i=i: e.activation(out=self.junk, in_=self.X[:, i, :], func=AF.Square,
                                                    accum_out=self.ss[:, i:i + 1]),
                 reads=[f"x{i}"], writes=["junk", "ss"])
        lnv = self.small[:, 0:16]
        P.op("act", lambda e: e.activation(out=lnv, in_=self.ss, func=AF.Ln, scale=1.0 / D, bias=EPS),
             reads=["ss"], writes=["lnv"])
        P.op("act", lambda e: e.activation(out=self.rstd, in_=lnv, func=AF.Exp, scale=-0.5), reads=["lnv"], writes=["rstd"])

    def init_small(self):
        self.P.op("dve", lambda e: e.memset(self.small[:, 60:61], EPS), writes=["small_eps"])

    def halo_exchange(self, idx, HT, xh, hh, xh_key, hh_key):
        P = self.P
        cin, cout = self.cc_h_in[idx], self.cc_h_out[idx]
        P.dma("sp", lambda e: e.dma_start(out=cin, in_=self.X[126:128, NT - 1, :]), reads=[f"x{NT-1}"], writes=[f"cch_in{idx}"])
        P.dma("pool", lambda e: e.collective_compute("AllGather", ALU.bypass, replica_groups=[list(range(NCORES))],
                                                     ins=[cin], outs=[cout]),
              reads=[f"cch_in{idx}"], writes=[f"cch_out{idx}"], inc=1)
        P.dma("sp", lambda e: e.dma_start(out=xh, in_=cout), reads=[f"cch_out{idx}"], writes=[xh_key])
        ssh = self.small[0:16, 20:21]
        lnh = self.small[0:16, 21:22]
        rsh = self.small[0:16, 22:23]
        P.op("act", lambda e: e.activation(out=self.junk[0:16, :], in_=xh, func=AF.Square, accum_out=ssh), reads=[xh_key], writes=["junk", "ssh"])
        P.op("act", lambda e: e.activation(out=lnh, in_=ssh, func=AF.Ln, scale=1.0 / D, bias=EPS),
             reads=["ssh"], writes=["lnh"])
        P.op("act", lambda e: e.activation(out=rsh, in_=lnh, func=AF.Exp, scale=-0.5), reads=["lnh"], writes=["rsh"])
        P.op("dve", lambda e: e.scalar_tensor_tensor(out=hh, in0=xh, scalar=rsh, in1=self.gbc[0:16, :], op0=ALU.mult, op1=ALU.mult),
             reads=[xh_key, "rsh", "gbc"], writes=[hh_key])
        pb, pk = self.fbank()
        for k in range(8):
            P.op("pe", lambda e, k=k: e.matmul(pb[:, 2 * k:2 * k + 2], lhsT=hh[:, k * 128:(k + 1) * 128], rhs=self.selb,
                                               start=True, stop=True),
                 reads=[hh_key, "selb"], writes=[pk])
        P.op("act", lambda e: e.activation(out=HT[:, :, 0:2], in_=pb[:, 0:16].rearrange("p (k t) -> p k t", t=2), func=AF.Copy),
             reads=[pk], writes=["HT_halo"])

    def build_hT(self, HT, hb, hb_keys):
        P = self.P
        self.norm_stats()
        for i in range(NT):
            b = i % 2
            P.op("dve", lambda e, i=i, b=b: e.scalar_tensor_tensor(out=hb[b], in0=self.X[:, i, :], scalar=self.rstd[:, i:i + 1],
                                                                     in1=self.gbc, op0=ALU.mult, op1=ALU.mult),
                 reads=[f"x{i}", "rstd", "gbc"], writes=[hb_keys[b]])
            pb, pk = self.bbank()
            for k in range(8):
                P.op("pe", lambda e, k=k, b=b, pb=pb: e.transpose(out=pb[:, k * 128:(k + 1) * 128], in_=hb[b][:, k * 128:(k + 1) * 128],
                                                                  identity=self.identb),
                     reads=[hb_keys[b], "identb"], writes=[pk])
            P.op("act", lambda e, i=i, pb=pb: e.activation(out=HT[:, :, 2 + i * 128:2 + (i + 1) * 128],
                                                            in_=pb.rearrange("p (k t) -> p k t", t=128), func=AF.Copy),
                 reads=[pk], writes=[f"HT{i // 4}"])

    def proj_group(self, HT, wT, wkey, tbp, halo_slot, stage, stage_key, tail, tail_key, scaled=None):
        P = self.P
        (pa, ka), (pb, kb) = self.fbank2()
        hb_, hk_ = self.halo_bank
        for k in range(8):
            for t, (pp, kk) in enumerate(((pa, ka), (pb, kb))):
                c0 = 2 + (tbp * 2 + t) * 512
                P.op("pe", lambda e, k=k, pp=pp, c0=c0: e.matmul(pp, lhsT=wT(k), rhs=HT[:, k, c0:c0 + 512], start=(k == 0), stop=(k == 7)),
                     reads=[wkey, f"HT{tbp * 2 + t}"], writes=[kk])
            if tbp == 0 and halo_slot is not None:
                P.op("pe", lambda e, k=k: e.matmul(hb_[:, 2 * halo_slot:2 * halo_slot + 2], lhsT=wT(k), rhs=HT[:, k, 0:2],
                                                   start=(k == 0), stop=(k == 7)),
                     reads=[wkey, "HT_halo"], writes=[hk_])
        if halo_slot is not None:
            if tbp == 0:
                P.op("act", lambda e: e.activation(out=stage[:, 0:2], in_=hb_[:, 2 * halo_slot:2 * halo_slot + 2], func=AF.Copy),
                     reads=[hk_], writes=[stage_key])
            else:
                P.op("act", lambda e: e.activation(out=stage[:, 0:2], in_=tail, func=AF.Copy), reads=[tail_key], writes=[stage_key])
        for t, (pp, kk) in enumerate(((pa, ka), (pb, kb))):
            P.op("act", lambda e, pp=pp, t=t: e.activation(out=stage[:, 2 + t * 512:2 + (t + 1) * 512], in_=pp, func=AF.Copy),
                 reads=[kk], writes=[stage_key])
            if scaled is not None:
                sbuf, skey, sc = scaled
                P.op("act", lambda e, pp=pp, t=t: e.activation(out=sbuf[:, t * 512:(t + 1) * 512], in_=pp, func=AF.Copy, scale=sc),
                     reads=[kk, "convw"], writes=[skey])
        if halo_slot is not None and tbp == 0:
            P.op("act", lambda e: e.activation(out=tail, in_=stage[:, 1024:1026], func=AF.Copy), reads=[stage_key], writes=[tail_key])

    def out_proj_add(self, lhs_of, lhs_keys, nk, w_of, wkey, tiles):
        P = self.P
        for i in tiles:
            for n in range(2):
                pp, kk = self.fbank()
                for k in range(nk):
                    P.op("pe", lambda e, k=k, i=i, n=n, pp=pp: e.matmul(pp, lhsT=lhs_of(k, i), rhs=w_of(k, n), start=(k == 0), stop=(k == nk - 1)),
                         reads=list(lhs_keys(i)) + [wkey], writes=[kk])
                P.op("dve", lambda e, i=i, n=n, pp=pp: e.tensor_tensor(out=self.X[:, i, n * 512:(n + 1) * 512], in0=pp,
                                                                         in1=self.X[:, i, n * 512:(n + 1) * 512], op=ALU.add),
                     reads=[kk, f"x{i}"], writes=[f"x{i}"])

    def ffn(self, layer, halo_idx):
        P = self.P
        P.barrier()
        self.arena_reset()
        HT = self.sb("HT", [128, 8, 2 + TOK], BF16)
        hid = self.sb("hid", [128, 11, TOK], BF16)
        wd = self.sb("wd", [128, 11, D], BF16)
        wup = [self.sb(f"wup{i}", [128, 8, 256], BF16) for i in range(2)]
        stage = [self.sb(f"stage{i}", [128, 1026], F32) for i in range(3)]
        t2_off = [None, None]
        t2 = []
        for i in range(2):
            t2_off[i] = (self.sb_ptr + 31) // 32 * 32
            t2.append(self.sb(f"t2_{i}", [128, 1024], F32))
        sa_off = [None, None]
        sa = []
        for i in range(2):
            sa_off[i] = (self.sb_ptr + 31) // 32 * 32
            sa.append(self.sb(f"sa{i}", [128, 1024], BF16))
        tails = self.sb("tails", [128, 2, 2], F32)
        xh = self.sb("xh", [16, D], F32, at=t2_off[0])
        hh = self.sb("hh", [16, D], BF16, at=sa_off[0])
        hb = sa
        hb_keys = ["sa0", "sa1"]

        self.load_gbc(self.norm_ffn_g[layer])
        cw = self.ffn_conv_w[layer]
        for q in range(4):
            P.dma("sp", lambda e, q=q: e.dma_start(out=self.convw[:, q * 11:(q + 1) * 11, :],
                                                   in_=cw[q * 1408:(q + 1) * 1408, :].rearrange("(m p) c -> p m c", p=128)),
                  writes=["convw"])
        self.halo_exchange(halo_idx, HT, xh, hh, "t2_0", "sa0")
        self.build_hT(HT, hb, hb_keys)

        wu = self.ffn_w_up[layer]
        wdn = self.ffn_w_down[layer]

        def load_pair(hd, j, b):
            m = hd * 11 + j
            P.dma("pool", lambda e: e.dma_start(out=wup[b][:, :, 0:128],
                                                in_=wu[:, m * 128:(m + 1) * 128].rearrange("(k p) n -> p k n", p=128)),
                  writes=[f"wup{b}a"])
            P.dma("pool", lambda e: e.dma_start(out=wup[b][:, :, 128:256],
                                                in_=wu[:, DFF + m * 128:DFF + (m + 1) * 128].rearrange("(k p) n -> p k n", p=128)),
                  writes=[f"wup{b}u"])

        pairs = [(hd, j) for hd in range(2) for j in range(11)]
        load_pair(0, 0, 0)
        st_i = 0
        for pi, (hd, j) in enumerate(pairs):
            b = pi % 2
            if pi + 1 < len(pairs):
                load_pair(pairs[pi + 1][0], pairs[pi + 1][1], (pi + 1) % 2)
            if j == 0:
                for q in range(2):
                    r0 = (hd * 11 + q * 6) * 128
                    nk = 6 if q == 0 else 5
                    P.dma("pool", lambda e, r0=r0, nk=nk, q=q: e.dma_start(
                        out=wd[:, q * 6:q * 6 + nk, :], in_=wdn[r0:r0 + nk * 128, :].rearrange("(k p) n -> p k n", p=128)),
                        writes=["wd"])
            m = hd * 11 + j
            for tbp in range(2):
                ybuf = {}
                for wi, which in enumerate("au"):
                    mm = m if which == "a" else 22 + m
                    stg = stage[st_i % 3]
                    skey = f"stage{st_i % 3}"
                    st_i += 1
                    tb = t2[wi]
                    tkey = f"t2_{wi}"
                    self.proj_group(HT, lambda k, b=b, wi=wi: wup[b][:, k, wi * 128:(wi + 1) * 128], f"wup{b}{which}", tbp,
                                    (j * 2 + wi) if True else None, stg, skey, tails[:, wi, :], f"tail{wi}",
                                    scaled=(tb, tkey, self.convw[:, mm, 2:3]))
                    P.op("dve", lambda e, stg=stg, tb=tb, mm=mm: e.scalar_tensor_tensor(
                        out=tb, in0=stg[:, 1:1025], scalar=self.convw[:, mm, 1:2], in1=tb, op0=ALU.mult, op1=ALU.add),
                        reads=[skey, tkey, "convw"], writes=[tkey])
                    P.op("dve", lambda e, stg=stg, tb=tb, mm=mm: e.scalar_tensor_tensor(
                        out=tb, in0=stg[:, 0:1024], scalar=self.convw[:, mm, 0:1], in1=tb, op0=ALU.mult, op1=ALU.add),
                        reads=[skey, tkey, "convw"], writes=[tkey])
                    ybuf[which] = (tb, tkey)
                sb_ = sa[(pi * 2 + tbp) % 2]
                sk_ = f"sa{(pi * 2 + tbp) % 2}"
                P.op("act", lambda e, sb_=sb_: e.activation(out=sb_, in_=ybuf["a"][0], func=AF.Silu), reads=[ybuf["a"][1]], writes=[sk_])
                P.op("dve", lambda e, sb_=sb_, j=j, tbp=tbp: e.tensor_tensor(out=hid[:, j, tbp * 1024:(tbp + 1) * 1024], in0=sb_,
                                                                             in1=ybuf["u"][0], op=ALU.mult),
                     reads=[sk_, ybuf["u"][1]], writes=[f"hid{tbp}"])
            if j == 10:
                self.out_proj_add(lambda k, i: hid[:, k, i * 128:(i + 1) * 128], lambda i: [f"hid{i // 8}"], 11,
                                  lambda k, n: wd[:, k, n * 512:(n + 1) * 512], "wd", range(NT))

    def sconv(self, halo_idx):
        P = self.P
        P.barrier()
        self.arena_reset()
        HT = self.sb("HT", [128, 8, 2 + TOK], BF16)
        win = self.sb("scwin", [128, 8, 3072], BF16)
        wout = self.sb("scwout", [128, 8, D], BF16)
        yT = self.sb("yT", [128, 8, 1024], BF16)
        stage = [self.sb(f"cst{i}", [128, 1026], F32) for i in range(2)]
        cu_off = (self.sb_ptr + 31) // 32 * 32
        cu = self.sb("cu", [128, 1026], F32)
        tb_off = (self.sb_ptr + 31) // 32 * 32
        tb = self.sb("tb", [128, 1024], F32)
        braw_off = (self.sb_ptr + 31) // 32 * 32
        braw = self.sb("braw", [128, 1026], F32)
        hb = [self.sb(f"hb{i}", [128, D], BF16, at=braw_off + 2048 * i) for i in range(2)]
        tails = self.sb("sctails", [128, 8, 2], F32)
        xh = self.sb("xh", [16, D], F32, at=tb_off)
        hh = self.sb("hh", [16, D], BF16, at=cu_off)

        self.load_gbc(self.norm_mix_g[1])
        P.dma("sp", lambda e: e.dma_start(out=self.convw[:, 0:8, :], in_=self.sc_conv_w[0].rearrange("(m p) c -> p m c", p=128)),
              writes=["convw"])
        wi_ = self.sc_w_in[0]
        for q in range(6):
            P.dma("pool", lambda e, q=q: e.dma_start(out=win[:, :, q * 512:(q + 1) * 512],
                                                     in_=wi_[:, q * 512:(q + 1) * 512].rearrange("(k p) n -> p k n", p=128)),
                  writes=[f"scwin{q}"])
        for q in range(2):
            P.dma("pool", lambda e, q=q: e.dma_start(out=wout[:, q * 4:(q + 1) * 4, :],
                                                     in_=self.sc_w_out[0][q * 512:(q + 1) * 512, :].rearrange("(k p) n -> p k n", p=128)),
                  writes=["scwout"])
        self.halo_exchange(halo_idx, HT, xh, hh, "tb", "cu")
        self.build_hT(HT, hb, ["hb0", "hb1"])
        for tbp in range(2):
            for j in range(8):
                self.proj_group(HT, lambda k, j=j: win[:, k, 1024 + j * 128:1024 + (j + 1) * 128], f"scwin{(1024 + j * 128) // 512}", tbp,
                                j * 2, stage[0], "cst0", self.small[:, 40:42], "junk_tail0")
                self.proj_group(HT, lambda k, j=j: win[:, k, 2048 + j * 128:2048 + (j + 1) * 128], f"scwin{(2048 + j * 128) // 512}", tbp,
                                j * 2 + 1, stage[1], "cst1", self.small[:, 42:44], "junk_tail1")
                if tbp == 1:
                    pass
                P.op("dve", lambda e: e.tensor_tensor(out=cu, in0=stage[0], in1=stage[1], op=ALU.mult), reads=["cst0", "cst1"], writes=["cu"])
                if tbp == 1:
                    P.op("dve", lambda e, j=j: e.tensor_copy(out=cu[:, 0:2], in_=tails[:, j, :]), reads=["sctails"], writes=["cu"])
                else:
                    P.op("dve", lambda e, j=j: e.tensor_copy(out=tails[:, j, :], in_=cu[:, 1024:1026]), reads=["cu"], writes=["sctails"])
                P.op("dve", lambda e, j=j: e.tensor_scalar(out=tb, in0=cu[:, 2:1026], scalar1=self.convw[:, j, 2:3], scalar2=None, op0=ALU.mult),
                     reads=["cu", "convw"], writes=["tb"])
                P.op("dve", lambda e, j=j: e.scalar_tensor_tensor(out=tb, in0=cu[:, 1:1025], scalar=self.convw[:, j, 1:2], in1=tb,
                                                                   op0=ALU.mult, op1=ALU.add), reads=["cu", "tb", "convw"], writes=["tb"])
                P.op("dve", lambda e, j=j: e.scalar_tensor_tensor(out=tb, in0=cu[:, 0:1024], scalar=self.convw[:, j, 0:1], in1=tb,
                                                                   op0=ALU.mult, op1=ALU.add), reads=["cu", "tb", "convw"], writes=["tb"])
                self.proj_group(HT, lambda k, j=j: win[:, k, j * 128:(j + 1) * 128], f"scwin{(j * 128) // 512}", tbp,
                                None, braw, "braw", None, None)
                P.op("dve", lambda e, j=j: e.tensor_tensor(out=yT[:, j, :], in0=braw[:, 2:1026], in1=tb, op=ALU.mult),
                     reads=["braw", "tb"], writes=["yT"])
            self.out_proj_add(lambda k, i, tbp=tbp: yT[:, k, (i - tbp * 8) * 128:(i - tbp * 8 + 1) * 128], lambda i: ["yT"], 8,
                              lambda k, n: wout[:, k, n * 512:(n + 1) * 512], "scwout", range(tbp * 8, tbp * 8 + 8))

    def gla(self):
        P = self.P
        self.arena_reset()
        win = self.sb("gwin", [128, 8, 3088], BF16)
        wout = self.sb("gwout", [128, 8, D], BF16)
        wg = self.sb("gwg", [32, 512], BF16)
        hng = self.sb("hng", [128, 8], F32)
        hb = [self.sb(f"ghb{i}", [128, D], BF16) for i in range(2)]
        hTt = [self.sb(f"ghT{i}", [128, 8, 128], BF16) for i in range(2)]
        aT = [self.sb(f"gaT{i}", [32, 128], BF16) for i in range(2)]
        ebuf = self.sb("gE", [128, 512], F32)
        lbuf = self.sb("gL", [128, 512], F32)
        Eb = [self.sb(f"gEb{i}", [128, 512], F32) for i in range(2)]
        Enb = [self.sb(f"gEnb{i}", [128, 512], F32) for i in range(2)]
        Er = [self.sb(f"gEr{i}", [128, 512], F32) for i in range(2)]
        dec = [self.sb(f"gdec{i}", [128, 4], F32) for i in range(3)]
        kdec = [self.sb(f"gkdec{i}", [128, 512], BF16) for i in range(2)]
        kin = [self.sb(f"gkin{i}", [128, 512], BF16) for i in range(2)]
        qin = [self.sb(f"gqin{i}", [128, 512], BF16) for i in range(2)]
        vb = [self.sb(f"gvb{i}", [128, D], BF16) for i in range(2)]
        qkT = [self.sb(f"gqkT{i}", [128, 8, 128], BF16) for i in range(2)]
        scm = [self.sb(f"gscm{i}", [128, 4, 128], BF16) for i in range(2)]
        S32 = self.sb("gS32", [128, 4, 256], F32)
        Sb = self.sb("gSb", [128, 4, 256], BF16)
        bsum = self.sb("gbsum", [128, 4], F32)
        ssq = [self.sb(f"gssq{i}", [128, 4], F32) for i in range(2)]
        rsq = [self.sb(f"grsq{i}", [128, 8], F32) for i in range(2)]
        sg_off = (self.sb_ptr + 31) // 32 * 32
        sg = self.sb("gsg", [128, D], F32)
        gg = [self.sb(f"ggg{i}", [128, D], BF16) for i in range(2)]
        og = [self.sb(f"gog{i}", [128, D], BF16) for i in range(2)]
        ogT = [self.sb(f"gogT{i}", [128, 8, 128], BF16) for i in range(2)]
        rk_off = (self.sb_ptr + 31) // 32 * 32
        rk = self.sb("grk", [128, 1028], F32)
        wstage = [self.sb("wstage0", [128, D], F32, at=rk_off), self.sb("wstage1", [128, D], F32, at=sg_off)]
        wstage_keys = ["grk", "gsg"]
        Dp = self.sb("gDp", [128, 4], F32)

        pO_banks = [(self.psf[3], "psf3"), (self.psf[4], "psf4")]
        self.rot = [(self.psf[0], "psf0"), (self.psf[1], "psf1"), (self.psf[2], "psf2"), self.halo_bank]
        self.psf_i = 0

        wi_ = self.gla_w_in[0]

        def load_win(q):
            if q == 6:
                P.dma("pool", lambda e: e.dma_start(out=win[:, :, 3072:3088], in_=wi_[:, 3072:3088].rearrange("(k p) n -> p k n", p=128)),
                      writes=["gwin6"])
            else:
                P.dma("pool", lambda e, q=q: e.dma_start(out=win[:, :, q * 512:(q + 1) * 512],
                                                         in_=wi_[:, q * 512:(q + 1) * 512].rearrange("(k p) n -> p k n", p=128)),
                      writes=[f"gwin{q}"])

        load_win(6)
        P.dma("pool", lambda e: e.dma_start(out=wg[0:16, :], in_=self.gla_w_gate_up[0]), writes=["gwg"])
        P.dma("pool", lambda e: e.dma_start(out=wg[16:17, :], in_=self.gla_b_gate[0:1, :]), writes=["gwg"])
        for q in (1, 2, 3):
            load_win(q)
        self.load_gbc(self.norm_mix_g[0])
        for b in range(2):
            P.op("dve", lambda e, b=b: e.memset(aT[b], 1.0), writes=[f"gaT{b}"])
        P.op("dve", lambda e: e.memset(S32, 0.0), writes=["S32"])
        P.op("dve", lambda e: e.memset(bsum, 0.0), writes=["bsum"])
        self.norm_stats()

        def late_weights():
            for q in (0, 4, 5):
                load_win(q)
            P.dma("sp", lambda e: e.dma_start(out=hng, in_=self.gla_hn_g[0].rearrange("h e -> (h e)").rearrange("(k p) -> p k", p=128),
                                              allow_slow_non_contiguous=True), writes=["hng"])
            for k in range(8):
                b = k % 2
                P.dma("sp", lambda e, k=k, b=b: e.dma_start(out=wstage[b], in_=self.gla_w_out[0][k * 128:(k + 1) * 128, :]),
                      writes=[wstage_keys[b]])
                P.op("dve", lambda e, k=k, b=b: e.tensor_scalar(out=wout[:, k, :], in0=wstage[b], scalar1=hng[:, k:k + 1], scalar2=None,
                                                                op0=ALU.mult), reads=[wstage_keys[b], "hng"], writes=["gwout"])

        def inproj(i, c0):
            hT = hTt[i % 2]
            pp, kk = self.fbank()
            for k in range(8):
                P.op("pe", lambda e, k=k, pp=pp, hT=hT, c0=c0: e.matmul(pp, lhsT=hT[:, k, :], rhs=win[:, k, c0:c0 + 512],
                                                                        start=(k == 0), stop=(k == 7)),
                     reads=[f"ghT{i % 2}", f"gwin{c0 // 512}"], writes=[kk])
            return pp, kk

        def A0(i):
            b = i % 2
            P.op("dve", lambda e, i=i, b=b: e.scalar_tensor_tensor(out=hb[b], in0=self.X[:, i, :], scalar=self.rstd[:, i:i + 1],
                                                                     in1=self.gbc, op0=ALU.mult, op1=ALU.mult),
                 reads=[f"x{i}", "rstd", "gbc"], writes=[f"ghb{b}"])

        def A1(i):
            b = i % 2
            pb, pk = self.bbank()
            for k in range(8):
                P.op("pe", lambda e, k=k, b=b, pb=pb: e.transpose(out=pb[:, k * 128:(k + 1) * 128], in_=hb[b][:, k * 128:(k + 1) * 128],
                                                                  identity=self.identb),
                     reads=[f"ghb{b}", "identb"], writes=[pk])
            P.op("act", lambda e, b=b, pb=pb: e.activation(out=hTt[b], in_=pb.rearrange("p (k t) -> p k t", t=128), func=AF.Copy),
                 reads=[pk], writes=[f"ghT{b}"])

        def A2(i):
            b = i % 2
            hT = hTt[b]
            pm, pmk = self.fbank()
            for k in range(8):
                P.op("pe", lambda e, k=k, hT=hT, pm=pm: e.matmul(pm[0:16, 0:128], lhsT=win[:, k, 3072:3088], rhs=hT[:, k, :],
                                                                 start=(k == 0), stop=(k == 7)),
                     reads=["gwin6", f"ghT{b}"], writes=[pmk])
            P.op("act", lambda e, b=b, pm=pm: e.activation(out=aT[b][0:16, :], in_=pm[0:16, 0:128], func=AF.Copy), reads=[pmk], writes=[f"gaT{b}"])

        def A3(i):
            b = i % 2
            pz, pzk = self.fbank()
            P.op("pe", lambda e, b=b, pz=pz: e.matmul(pz, lhsT=aT[b][0:17, :], rhs=wg[0:17, :], start=True, stop=True),
                 reads=[f"gaT{b}", "gwg"], writes=[pzk])
            P.op("act", lambda e, pz=pz: e.activation(out=ebuf, in_=pz, func=AF.Exp, scale=-1.0), reads=[pzk], writes=["gE"])
            P.op("act", lambda e: e.activation(out=lbuf, in_=ebuf, func=AF.Ln, bias=1.0), reads=["gE"], writes=["gL"])

        def A4(i, p2):
            b = i % 2
            pr, prk = self.fbank()
            P.op("pe", lambda e, pr=pr: e.matmul(pr, lhsT=self.tri_rev, rhs=lbuf, start=True, stop=True), reads=["cst", "gL"], writes=[prk])
            pd, pdk = self.fbank()
            for h in range(4):
                P.op("pe", lambda e, h=h, pd=pd: e.matmul(pd[:, h:h + 1], lhsT=lbuf[:, h * 128:(h + 1) * 128], rhs=self.negcol,
                                                          start=True, stop=True), reads=["cst", "gL"], writes=[pdk])
            P.op("act", lambda e, b=b, pr=pr: e.activation(out=Er[b], in_=pr, func=AF.Exp), reads=[prk], writes=[f"gEr{b}"])
            P.op("act", lambda e, i=i, pd=pd: e.activation(out=dec[i % 3], in_=pd[:, 0:4], func=AF.Exp), reads=[pdk], writes=[f"gdec{i % 3}"])
            if not p2:
                P.op("dve", lambda e, pd=pd: e.tensor_tensor(out=bsum, in0=pd[:, 0:4], in1=bsum, op=ALU.add), reads=[pdk, "bsum"], writes=["bsum"])
            else:
                pbc, pbck = self.fbank()
                P.op("pe", lambda e, pbc=pbc: e.matmul(pbc, lhsT=self.tri_incl, rhs=lbuf, start=True, stop=True), reads=["cst", "gL"], writes=[pbck])
                P.op("act", lambda e, b=b, pbc=pbc: e.activation(out=Eb[b], in_=pbc, func=AF.Exp), reads=[pbck], writes=[f"gEb{b}"])
                P.op("act", lambda e, b=b, pbc=pbc: e.activation(out=Enb[b], in_=pbc, func=AF.Exp, scale=-1.0), reads=[pbck], writes=[f"gEnb{b}"])

        def B1(i, p2):
            b = i % 2
            pK, pKk = inproj(i, 512)
            P.op("dve", lambda e, b=b, pK=pK: e.tensor_tensor(out=kdec[b], in0=pK, in1=Er[b], op=ALU.mult), reads=[pKk, f"gEr{b}"], writes=[f"gkdec{b}"])
            if p2:
                P.op("dve", lambda e, b=b, pK=pK: e.tensor_tensor(out=kin[b], in0=pK, in1=Enb[b], op=ALU.mult), reads=[pKk, f"gEnb{b}"], writes=[f"gkin{b}"])

        def B2(i):
            b = i % 2
            pQ, pQk = inproj(i, 0)
            P.op("dve", lambda e, b=b, pQ=pQ: e.scalar_tensor_tensor(out=qin[b], in0=pQ, scalar=128.0 ** -0.5, in1=Eb[b],
                                                                      op0=ALU.mult, op1=ALU.mult), reads=[pQk, f"gEb{b}"], writes=[f"gqin{b}"])

        def BV(i, hv):
            b = i % 2
            pV, pVk = inproj(i, 1024 + hv * 512)
            P.op("act", lambda e, b=b, hv=hv, pV=pV: e.activation(out=vb[b][:, hv * 512:(hv + 1) * 512], in_=pV, func=AF.Copy),
                 reads=[pVk], writes=[f"gvb{b}"])

        def BG(i, hv):
            b = i % 2
            pG, pGk = inproj(i, 2048 + hv * 512)
            sl = slice(hv * 512, (hv + 1) * 512)
            P.op("act", lambda e, sl=sl, pG=pG: e.activation(out=sg[:, sl], in_=pG, func=AF.Exp, scale=-1.0), reads=[pGk], writes=["gsg"])
            P.op("act", lambda e, sl=sl: e.activation(out=sg[:, sl], in_=sg[:, sl], func=AF.Ln, bias=1.0), reads=["gsg"], writes=["gsg"])
            P.op("act", lambda e, sl=sl: e.activation(out=sg[:, sl], in_=sg[:, sl], func=AF.Exp, scale=-1.0), reads=["gsg"], writes=["gsg"])
            P.op("dve", lambda e, b=b, sl=sl, pG=pG: e.tensor_tensor(out=gg[b][:, sl], in0=pG, in1=sg[:, sl], op=ALU.mult),
                 reads=[pGk, "gsg"], writes=[f"ggg{b}"])

        def C1(i):
            b = i % 2
            pt, ptk = self.bbank()
            for h in range(4):
                P.op("pe", lambda e, h=h, b=b, pt=pt: e.transpose(out=pt[:, h * 128:(h + 1) * 128], in_=qin[b][:, h * 128:(h + 1) * 128],
                                                                  identity=self.identb), reads=[f"gqin{b}", "identb"], writes=[ptk])
            for h in range(4):
                P.op("pe", lambda e, h=h, b=b, pt=pt: e.transpose(out=pt[:, (4 + h) * 128:(5 + h) * 128], in_=kin[b][:, h * 128:(h + 1) * 128],
                                                                  identity=self.identb), reads=[f"gkin{b}", "identb"], writes=[ptk])
            P.op("dve", lambda e, b=b, pt=pt: e.tensor_copy(out=qkT[b], in_=pt.rearrange("p (k t) -> p k t", t=128)),
                 reads=[ptk], writes=[f"gqkT{b}"])

        def C2(i):
            b = i % 2
            psS, psSk = self.fbank()
            for h in range(4):
                P.op("pe", lambda e, h=h, b=b, psS=psS: e.matmul(psS[:, h * 128:(h + 1) * 128], lhsT=qkT[b][:, 4 + h, :], rhs=qkT[b][:, h, :],
                                                                 start=True, stop=True), reads=[f"gqkT{b}"], writes=[psSk])
            P.op("dve", lambda e, b=b, psS=psS: e.tensor_tensor(out=scm[b], in0=psS.rearrange("p (h c) -> p h c", c=128),
                                                                  in1=self.cmask.unsqueeze(1).broadcast_to([128, 4, 128]), op=ALU.mult),
                 reads=[psSk, "cst"], writes=[f"gscm{b}"])

        def C3(i):
            b = i % 2
            for hp in range(2):
                pp, kk = pO_banks[hp]
                for hh_ in range(2):
                    h = hp * 2 + hh_
                    oc = slice(hh_ * 256, (hh_ + 1) * 256)
                    P.op("pe", lambda e, h=h, b=b, pp=pp, oc=oc: e.matmul(pp[:, oc], lhsT=scm[b][:, h, :], rhs=vb[b][:, h * 256:(h + 1) * 256],
                                                                          start=True, stop=False), reads=[f"gscm{b}", f"gvb{b}"], writes=[kk])
                    P.op("pe", lambda e, h=h, b=b, pp=pp, oc=oc: e.matmul(pp[:, oc], lhsT=qkT[b][:, h, :], rhs=Sb[:, h, :],
                                                                          start=False, stop=True), reads=[f"gqkT{b}", "Sb"], writes=[kk])
            for hp in range(2):
                pp, kk = pO_banks[hp]
                for hh_ in range(2):
                    h = hp * 2 + hh_
                    oc = slice(hh_ * 256, (hh_ + 1) * 256)
                    P.op("act", lambda e, h=h, b=b, pp=pp, oc=oc: e.activation(out=self.junk[:, 0:256], in_=pp[:, oc], func=AF.Square,
                                                                               accum_out=ssq[b][:, h:h + 1]),
                         reads=[kk], writes=["junk", f"gssq{b}"])
            P.op("act", lambda e, b=b: e.activation(out=rsq[b][:, 0:4], in_=ssq[b], func=AF.Ln, scale=1.0 / 256, bias=EPS),
                 reads=[f"gssq{b}"], writes=[f"grsqa{b}"])
            P.op("act", lambda e, b=b: e.activation(out=rsq[b][:, 4:8], in_=rsq[b][:, 0:4], func=AF.Exp, scale=-0.5),
                 reads=[f"grsqa{b}"], writes=[f"grsq{b}"])

        def C4(i, p2):
            b = i % 2
            for hp in range(2):
                pp, kk = self.fbank()
                for hh_ in range(2):
                    h = hp * 2 + hh_
                    oc = slice(hh_ * 256, (hh_ + 1) * 256)
                    P.op("pe", lambda e, h=h, b=b, pp=pp, oc=oc: e.matmul(pp[:, oc], lhsT=kdec[b][:, h * 128:(h + 1) * 128],
                                                                          rhs=vb[b][:, h * 256:(h + 1) * 256], start=True, stop=True),
                         reads=[f"gkdec{b}", f"gvb{b}"], writes=[kk])
                for hh_ in range(2):
                    h = hp * 2 + hh_
                    oc = slice(hh_ * 256, (hh_ + 1) * 256)
                    P.op("dve", lambda e, h=h, i=i, pp=pp, oc=oc: e.scalar_tensor_tensor(out=S32[:, h, :], in0=S32[:, h, :], scalar=dec[i % 3][:, h:h + 1],
                                                                                         in1=pp[:, oc], op0=ALU.mult, op1=ALU.add),
                         reads=["S32", f"gdec{i % 3}", kk], writes=["S32"])
            if p2:
                P.op("act", lambda e: e.activation(out=Sb, in_=S32, func=AF.Copy), reads=["S32"], writes=["Sb"])

        def C5(i):
            b = i % 2
            for hp in range(2):
                pp, kk = pO_banks[hp]
                for hh_ in range(2):
                    h = hp * 2 + hh_
                    oc = slice(hh_ * 256, (hh_ + 1) * 256)
                    P.op("dve", lambda e, h=h, b=b, pp=pp, oc=oc: e.scalar_tensor_tensor(
                        out=og[b][:, h * 256:(h + 1) * 256], in0=pp[:, oc], scalar=rsq[b][:, 4 + h:5 + h], in1=gg[b][:, h * 256:(h + 1) * 256],
                        op0=ALU.mult, op1=ALU.mult), reads=[kk, f"grsq{b}", f"ggg{b}"], writes=[f"gog{b}"])
            pt, ptk = self.bbank()
            for k in range(8):
                P.op("pe", lambda e, k=k, b=b, pt=pt: e.transpose(out=pt[:, k * 128:(k + 1) * 128], in_=og[b][:, k * 128:(k + 1) * 128],
                                                                  identity=self.identb), reads=[f"gog{b}", "identb"], writes=[ptk])
            P.op("act", lambda e, b=b, pt=pt: e.activation(out=ogT[b], in_=pt.rearrange("p (k t) -> p k t", t=128), func=AF.Copy),
                 reads=[ptk], writes=[f"gogT{b}"])

        def C6(i):
            b = i % 2
            self.out_proj_add(lambda k, i_, b=b: ogT[b][:, k, :], lambda i_, b=b: [f"gogT{b}"], 8,
                              lambda k, n: wout[:, k, n * 512:(n + 1) * 512], "gwout", [i])

        def gla_pass(p2):
            A0(0)
            for t in range(NT + 2):
                a, bb, c = t, t - 1, t - 2
                va, vbb, vc = (0 <= a < NT), (0 <= bb < NT), (0 <= c < NT)
                if va: A1(a)
                if vc and p2: C1(c)
                if vbb: B1(bb, p2)
                if va: A2(a)
                if vc and p2: C2(c)
                if vbb and p2: B2(bb)
                if va: A3(a)
                if vc and p2: C3(c)
                if vbb: BV(bb, 0)
                if vc: C4(c, p2)
                if vbb: BV(bb, 1)
                if va: A4(a, p2)
                if a + 1 < NT: A0(a + 1)
                if vc and p2: C5(c)
                if vbb and p2: BG(bb, 0)
                if vbb and p2: BG(bb, 1)
                if vc and p2: C6(c)

        gla_pass(False)
        late_weights()
        P.op("act", lambda e: e.activation(out=Dp, in_=bsum, func=AF.Exp), reads=["bsum"], writes=["Dp"])
        P.dma("sp", lambda e: e.dma_start(out=self.cc_st_in[:, 0:1024], in_=S32.rearrange("p h e -> p (h e)")), reads=["S32"], writes=["ccst_in"])
        P.dma("sp", lambda e: e.dma_start(out=self.cc_st_in[:, 1024:1028], in_=Dp), reads=["Dp"], writes=["ccst_in"])
        P.dma("pool", lambda e: e.collective_compute("AllGather", ALU.bypass, replica_groups=[list(range(NCORES))],
                                                     ins=[self.cc_st_in], outs=[self.cc_st_out]),
              reads=["ccst_in"], writes=["ccst_out"], inc=1)
        P.op("dve", lambda e: e.memset(S32, 0.0), reads=["S32"], writes=["S32"])
        for j in range(NCORES - 1):
            P.dma("sp", lambda e, j=j: e.dma_start(out=rk, in_=self.cc_st_out[j * 128:(j + 1) * 128, :]), reads=["ccst_out"], writes=["grk"])
            P.op("dve", lambda e, j=j: e.tensor_scalar(out=Dp, in0=rk[:, 1024:1028], scalar1=self.mj[:, j:j + 1], scalar2=self.omj[:, j:j + 1],
                                                       op0=ALU.mult, op1=ALU.add), reads=["grk", "cst"], writes=["Dp"])
            P.op("dve", lambda e, j=j: e.tensor_scalar(out=rk[:, 0:1024], in0=rk[:, 0:1024], scalar1=self.mj[:, j:j + 1], scalar2=None,
                                                       op0=ALU.mult), reads=["grk", "cst"], writes=["grk"])
            for h in range(4):
                P.op("dve", lambda e, h=h: e.scalar_tensor_tensor(out=S32[:, h, :], in0=S32[:, h, :], scalar=Dp[:, h:h + 1],
                                                                   in1=rk[:, h * 256:(h + 1) * 256], op0=ALU.mult, op1=ALU.add),
                     reads=["S32", "Dp", "grk"], writes=["S32"])
        P.op("act", lambda e: e.activation(out=Sb, in_=S32, func=AF.Copy), reads=["S32"], writes=["Sb"])
        gla_pass(True)
        self.rot = None
        self.psf_i = 0

    def final(self):
        P = self.P
        P.barrier()
        self.arena_reset()
        ob = [self.sb(f"ob{i}", [128, D], F32) for i in range(4)]
        self.load_gbc(self.final_g[0])
        self.norm_stats()
        ov = self.out_d.rearrange("(n p) d -> p n d", p=128)
        fins = []
        for i in range(NT):
            b = i % 4
            P.op("dve", lambda e, i=i, b=b: e.scalar_tensor_tensor(out=ob[b], in0=self.X[:, i, :], scalar=self.rstd[:, i:i + 1], in1=self.gbc,
                                                                     op0=ALU.mult, op1=ALU.mult), reads=[f"x{i}", "rstd", "gbc"], writes=[f"ob{b}"])
            fins.append(P.dma("sp", lambda e, i=i, b=b: e.dma_start(out=ov[:, i, :], in_=ob[b]), reads=[f"ob{b}"], writes=[f"out{i}"]))
        return fins

    def build(self):
        self.load_consts()
        self.load_x()
        st = self.stages
        if st >= 1:
            self.gla()
        if st >= 2:
            self.ffn(0, 0)
        if st >= 3:
            self.sconv(1)
        if st >= 4:
            self.ffn(1, 2)
        fins = self.final()
        self.P.emit(final_waits=fins)
        return self.nc


def make_consts(core):
    c = np.zeros((128, 640), np.float32)
    c[:, 0:128] = np.eye(128, dtype=np.float32)
    s = np.arange(128)[:, None]
    t = np.arange(128)[None, :]
    c[:, 128:256] = np.where(s <= t, -1.0 / 16, 0.0)
    c[:, 256:384] = np.where(s > t, -1.0 / 16, 0.0)
    c[:, 384:512] = np.where(s <= t, 1.0, 0.0)
    c[:, 512] = -1.0 / 16
    for j in range(8):
        m = 1.0 if (j // 4 == core // 4 and j < core) else 0.0
        c[:, 513 + j] = m
        c[:, 536 + j] = 1.0 - m
    if core % 4 != 0:
        c[(core - 1) * 2 + 0, 528] = 1.0
        c[(core - 1) * 2 + 1, 529] = 1.0
    return c


_NC_CACHE = {}


def kernel(**inputs):
    stages = int(inputs.pop("_stages", 4))
    if stages not in _NC_CACHE:
        _NC_CACHE[stages] = Builder(stages).build()
    nc = _NC_CACHE[stages]
    x = np.ascontiguousarray(np.asarray(inputs["x"], dtype=np.float32)).reshape(NCORES, TOK, D)
    shared = {}
    for k, v in inputs.items():
        if k == "x":
            continue
        a = np.ascontiguousarray(np.asarray(v, dtype=np.float32))
        if k == "final_norm_g":
            a = a.reshape(1, D)
        shared[k] = a
    in_maps = []
    for c in range(NCORES):
        m = dict(shared)
        m["x"] = x[c]
        m["cst"] = make_consts(c)
        in_maps.append(m)
    res = run_bass_kernel_spmd(nc, in_maps, core_ids=list(range(NCORES)))
    out = np.stack([np.asarray(r["out"]) for r in res.results], axis=0)
    return out.reshape(2, 8192, D).astype(np.float32)
```
